# Optimizing a Trainium2 kernel written in Bass

```python
import jax
import jax.numpy as jnp
from jax import lax
import numpy as np

D_MODEL = 2048
BATCH = 4
SEQ = 4096
DEPTH = 2

Q_BLOCK = 128
NORM_EPS = 1e-6
FOX_HEADS = 4
FOX_HEAD_DIM = 128
SB_HEADS = 4
SB_HEAD_DIM = 128
MLA_HEADS = 4
MLA_Q_LORA = 512
MLA_KV_LORA = 256
MLA_NOPE_DIM = 128
MLA_ROPE_DIM = 64
MLA_V_DIM = 128
ROPE_BASE = 10000.0
POOL_GROUPS = 4
POOL_GROUP_DIM = 128
POOL_WINDOWS = (2, 4, 8, 16)
N_BRANCHES = 4
BRANCH_WIDTH = 512
N_EXPERT_GROUPS = 4
EXPERTS_PER_GROUP = 8
N_EXPERTS = N_EXPERT_GROUPS * EXPERTS_PER_GROUP
EXPERT_TOP_K = 2
D_EXPERT = 1024
EXPERT_BLOCK = 128

FOX_QKV = 3 * FOX_HEADS * FOX_HEAD_DIM
FOX_F = FOX_HEADS
SB_QKV = 3 * SB_HEADS * SB_HEAD_DIM
POOL_IN = POOL_GROUPS * POOL_GROUP_DIM
GATE_IN = N_BRANCHES * D_MODEL
SPLITS = (FOX_QKV, FOX_F, SB_QKV, MLA_Q_LORA, MLA_KV_LORA, MLA_ROPE_DIM, POOL_IN, GATE_IN)
N_IN = FOX_QKV + FOX_F + SB_QKV + MLA_Q_LORA + MLA_KV_LORA + MLA_ROPE_DIM + POOL_IN + GATE_IN

kernel_name = "hybrid_fox_sb_mla_pool_hmoe_adaln"


def _rmsnorm(x, g):
    xf = x.astype(jnp.float32)
    y = xf * lax.rsqrt(jnp.mean(xf * xf, axis=-1, keepdims=True) + NORM_EPS)
    return (y * g.astype(jnp.float32)).astype(x.dtype)


def _heads(t, n_heads):
    b, s, _ = t.shape
    return t.reshape(b, s, n_heads, -1).transpose(0, 2, 1, 3)


def _merge_blocks(o):
    nb, b, h, qb, d = o.shape
    return o.transpose(1, 0, 3, 2, 4).reshape(b, nb * qb, h * d)


def _rope(x, pos):
    half = MLA_ROPE_DIM // 2
    inv = jnp.power(ROPE_BASE, -2.0 * jnp.arange(half, dtype=jnp.float32) / MLA_ROPE_DIM)
    ang = pos.astype(jnp.float32)[:, None] * inv[None, :]
    cos = jnp.cos(ang).astype(x.dtype)
    sin = jnp.sin(ang).astype(x.dtype)
    x1, x2 = x[..., :half], x[..., half:]
    return jnp.concatenate([x1 * cos - x2 * sin, x1 * sin + x2 * cos], axis=-1)


def _causal_softmax_attention(q, k, v, log_decay_cum=None):
    s_len = q.shape[2]
    scale = q.shape[-1] ** -0.5
    kpos = jnp.arange(s_len)

    def block(i):
        t0 = i * Q_BLOCK
        qb = lax.dynamic_slice_in_dim(q, t0, Q_BLOCK, axis=2)
        s = jnp.einsum('bhqd,bhkd->bhqk', qb, k).astype(jnp.float32) * scale
        if log_decay_cum is not None:
            fq = lax.dynamic_slice_in_dim(log_decay_cum, t0, Q_BLOCK, axis=2)
            s = s + fq[..., :, None] - log_decay_cum[..., None, :]
        qpos = t0 + jnp.arange(Q_BLOCK)
        s = jnp.where(qpos[:, None] >= kpos[None, :], s, -jnp.inf)
        p = jax.nn.softmax(s, axis=-1)
        return jnp.einsum('bhqk,bhkd->bhqd', p.astype(v.dtype), v)

    return _merge_blocks(lax.map(block, jnp.arange(s_len // Q_BLOCK)))


def _stick_breaking_attention(q, k, v):
    s_len = q.shape[2]
    scale = q.shape[-1] ** -0.5
    kpos = jnp.arange(s_len)

    def block(i):
        t0 = i * Q_BLOCK
        qb = lax.dynamic_slice_in_dim(q, t0, Q_BLOCK, axis=2)
        z = jnp.einsum('bhqd,bhkd->bhqk', qb, k).astype(jnp.float32) * scale
        qpos = t0 + jnp.arange(Q_BLOCK)
        strict = qpos[:, None] > kpos[None, :]
        log_1m = jnp.where(strict, jax.nn.log_sigmoid(-z), 0.0)
        log_stick = lax.cumsum(log_1m, axis=3, reverse=True) - log_1m
        a = jnp.where(strict, jnp.exp(jax.nn.log_sigmoid(z) + log_stick), 0.0)
        return jnp.einsum('bhqk,bhkd->bhqd', a.astype(v.dtype), v)

    return _merge_blocks(lax.map(block, jnp.arange(s_len // Q_BLOCK)))


def _multiscale_pool(u, w_pool, pool_scale):
    b, s_len, _ = u.shape
    ug = u.astype(jnp.float32).reshape(b, s_len, POOL_GROUPS, POOL_GROUP_DIM)
    cs = jnp.concatenate([jnp.zeros_like(ug[:, :1]), jnp.cumsum(ug, axis=1)], axis=1)
    hi = jnp.arange(1, s_len + 1)
    outs = []
    for g, w in enumerate(POOL_WINDOWS):
        lo = jnp.maximum(hi - w, 0)
        cnt = (hi - lo).astype(jnp.float32)[None, :, None]
        mean = (cs[:, hi, g] - cs[:, lo, g]) / cnt
        outs.append(mean - ug[:, :, g])
    pooled = jnp.stack(outs, axis=2).astype(u.dtype)
    y = jnp.einsum('bsgc,gcd->bsgd', pooled, w_pool).reshape(b, s_len, POOL_IN)
    return y * pool_scale


def _mixer(h, pos, w_in, b_forget, g_q_norm, w_uq, g_kv_norm, w_ukv,
           w_pool, pool_scale, w_branch, w_out):
    b, s_len, d = h.shape
    proj = h @ w_in
    offs = [int(o) for o in np.cumsum(SPLITS)[:-1]]
    fox_qkv, fox_f, sb_qkv, c_q, c_kv, k_rope, pool_u, gate_logits = jnp.split(proj, offs, axis=-1)

    fq, fk, fv = [_heads(t, FOX_HEADS) for t in jnp.split(fox_qkv, 3, axis=-1)]
    log_f = jax.nn.log_sigmoid(fox_f.astype(jnp.float32) + b_forget.astype(jnp.float32))
    log_f_cum = jnp.cumsum(log_f, axis=1).transpose(0, 2, 1)
    y_fox = _causal_softmax_attention(fq, fk, fv, log_f_cum)

    sq, sk, sv = [_heads(t, SB_HEADS) for t in jnp.split(sb_qkv, 3, axis=-1)]
    y_sb = _stick_breaking_attention(sq, sk, sv)

    q = _heads(_rmsnorm(c_q, g_q_norm) @ w_uq, MLA_HEADS)
    q = jnp.concatenate([q[..., :MLA_NOPE_DIM], _rope(q[..., MLA_NOPE_DIM:], pos)], axis=-1)
    kv = _heads(_rmsnorm(c_kv, g_kv_norm) @ w_ukv, MLA_HEADS)
    k_nope, v = kv[..., :MLA_NOPE_DIM], kv[..., MLA_NOPE_DIM:]
    kr = jnp.broadcast_to(_rope(k_rope, pos)[:, None], (b, MLA_HEADS, s_len, MLA_ROPE_DIM))
    k = jnp.concatenate([k_nope, kr], axis=-1)
    y_mla = _causal_softmax_attention(q, k, v)

    y_pool = _multiscale_pool(pool_u, w_pool, pool_scale)

    gates = jax.nn.sigmoid(gate_logits.reshape(b, s_len, N_BRANCHES, d))
    branches = (y_fox, y_sb, y_mla, y_pool)
    merged = gates[:, :, 0] * (branches[0] @ w_branch[0])
    for n in range(1, N_BRANCHES):
        merged = merged + gates[:, :, n] * (branches[n] @ w_branch[n])
    return merged @ w_out


def _hier_moe(h, w_rg, b_rg, w_re, b_re, w_gate, w_up, w_down):
    b, s_len, d = h.shape
    n_tok = b * s_len
    xt = h.reshape(n_tok, d)
    g_logits = (xt @ w_rg).astype(jnp.float32) + b_rg.astype(jnp.float32)
    g_top, g_idx = lax.top_k(g_logits, 1)
    p_group = jnp.exp(g_top[:, 0] - jax.nn.logsumexp(g_logits, axis=-1))
    e_logits = ((xt @ w_re).astype(jnp.float32) + b_re.astype(jnp.float32)).reshape(
        n_tok, N_EXPERT_GROUPS, EXPERTS_PER_GROUP)
    e_logits = jnp.take_along_axis(e_logits, g_idx[:, :, None], axis=1)[:, 0]
    top_p, top_i = lax.top_k(jax.nn.softmax(e_logits, axis=-1), EXPERT_TOP_K)
    weights = p_group[:, None] * top_p / jnp.sum(top_p, axis=-1, keepdims=True)

    expert_id = (g_idx * EXPERTS_PER_GROUP + top_i).reshape(-1)
    tok_id = jnp.repeat(jnp.arange(n_tok, dtype=jnp.int32), EXPERT_TOP_K)
    w_flat = weights.reshape(-1)
    m = expert_id.shape[0]

    order = jnp.argsort(expert_id)
    e_sorted = expert_id[order]
    counts = jnp.bincount(expert_id, length=N_EXPERTS).astype(jnp.int32)
    padded = ((counts + EXPERT_BLOCK - 1) // EXPERT_BLOCK) * EXPERT_BLOCK
    start = jnp.cumsum(counts) - counts
    pend = jnp.cumsum(padded)
    pstart = pend - padded
    dest = pstart[e_sorted] + jnp.arange(m, dtype=jnp.int32) - start[e_sorted]
    n_rows = m + N_EXPERTS * EXPERT_BLOCK
    n_blocks = n_rows // EXPERT_BLOCK
    x_buf = jnp.zeros((n_rows, d), h.dtype).at[dest].set(xt[tok_id[order]])
    w_buf = jnp.zeros((n_rows,), jnp.float32).at[dest].set(w_flat[order])
    t_buf = jnp.zeros((n_rows,), jnp.int32).at[dest].set(tok_id[order])
    block_start = jnp.arange(n_blocks, dtype=jnp.int32) * EXPERT_BLOCK
    block_expert = jnp.minimum(jnp.searchsorted(pend, block_start, side='right'),
                               N_EXPERTS - 1).astype(jnp.int32)

    def expert_block(args):
        xb, e = args
        return (jax.nn.silu(xb @ w_gate[e]) * (xb @ w_up[e])) @ w_down[e]

    y = lax.map(expert_block, (x_buf.reshape(n_blocks, EXPERT_BLOCK, d), block_expert))
    y = y.reshape(n_rows, d) * w_buf[:, None].astype(h.dtype)
    out = jax.ops.segment_sum(y, t_buf, num_segments=n_tok)
    return out.reshape(b, s_len, d)


def setup_inputs(seed: int = 0) -> dict:
    key = jax.random.key(seed)
    k = jax.random.split(key, 24)
    L, D = DEPTH, D_MODEL

    def nrm(kk, shape, scale):
        return jax.random.normal(kk, shape, jnp.float32) * scale

    col_scale = jnp.ones((N_IN,), jnp.float32).at[FOX_QKV:FOX_QKV + FOX_F].set(0.1)
    return {
        "x": nrm(k[0], (BATCH, SEQ, D), 1.0),
        "c": nrm(k[1], (BATCH, D), 1.0),
        "w_mod": nrm(k[2], (L, D, 6 * D), 0.3 * D ** -0.5),
        "b_mod": nrm(k[3], (L, 6 * D), 0.02),
        "g_norm1": 1.0 + nrm(k[4], (L, D), 0.1),
        "g_norm2": 1.0 + nrm(k[5], (L, D), 0.1),
        "w_in": nrm(k[6], (L, D, N_IN), D ** -0.5) * col_scale,
        "b_forget": jax.random.uniform(k[7], (L, FOX_HEADS), jnp.float32, 1.0, 4.0),
        "g_q_norm": 1.0 + nrm(k[8], (L, MLA_Q_LORA), 0.1),
        "w_uq": nrm(k[9], (L, MLA_Q_LORA, MLA_HEADS * (MLA_NOPE_DIM + MLA_ROPE_DIM)), MLA_Q_LORA ** -0.5),
        "g_kv_norm": 1.0 + nrm(k[10], (L, MLA_KV_LORA), 0.1),
        "w_ukv": nrm(k[11], (L, MLA_KV_LORA, MLA_HEADS * (MLA_NOPE_DIM + MLA_V_DIM)), MLA_KV_LORA ** -0.5),
        "w_pool": nrm(k[12], (L, POOL_GROUPS, POOL_GROUP_DIM, POOL_GROUP_DIM), POOL_GROUP_DIM ** -0.5),
        "pool_scale": 1.0 + nrm(k[13], (L, POOL_IN), 0.1),
        "w_branch": nrm(k[14], (L, N_BRANCHES, BRANCH_WIDTH, D), BRANCH_WIDTH ** -0.5),
        "w_out": nrm(k[15], (L, D, D), D ** -0.5),
        "w_route_group": nrm(k[16], (L, D, N_EXPERT_GROUPS), D ** -0.5),
        "b_route_group": nrm(k[17], (L, N_EXPERT_GROUPS), 0.01),
        "w_route_expert": nrm(k[18], (L, D, N_EXPERTS), D ** -0.5),
        "b_route_expert": nrm(k[19], (L, N_EXPERTS), 0.01),
        "w_gate": nrm(k[20], (L, N_EXPERTS, D, D_EXPERT), D ** -0.5),
        "w_up": nrm(k[21], (L, N_EXPERTS, D, D_EXPERT), D ** -0.5),
        "w_down": nrm(k[22], (L, N_EXPERTS, D_EXPERT, D), D_EXPERT ** -0.5),
        "g_final": 1.0 + nrm(k[23], (D,), 0.1),
    }


def reference(x, c, w_mod, b_mod, g_norm1, g_norm2, w_in, b_forget, g_q_norm, w_uq,
              g_kv_norm, w_ukv, w_pool, pool_scale, w_branch, w_out, w_route_group,
              b_route_group, w_route_expert, b_route_expert, w_gate, w_up, w_down, g_final):
    s_len = x.shape[1]
    pos = jnp.arange(s_len)
    c_act = jax.nn.silu(c)
    for l in range(DEPTH):
        mod = c_act @ w_mod[l] + b_mod[l]
        shift1, scale1, gate1, shift2, scale2, gate2 = [m[:, None, :] for m in jnp.split(mod, 6, axis=-1)]
        h = _rmsnorm(x, g_norm1[l]) * (1.0 + scale1) + shift1
        x = x + gate1 * _mixer(h, pos, w_in[l], b_forget[l], g_q_norm[l], w_uq[l],
                               g_kv_norm[l], w_ukv[l], w_pool[l], pool_scale[l],
                               w_branch[l], w_out[l])
        h = _rmsnorm(x, g_norm2[l]) * (1.0 + scale2) + shift2
        x = x + gate2 * _hier_moe(h, w_route_group[l], b_route_group[l], w_route_expert[l],
                                  b_route_expert[l], w_gate[l], w_up[l], w_down[l])
    return _rmsnorm(x, g_final)
```

```python
import numpy as np
from contextlib import ExitStack
import concourse.bass as bass
import concourse.mybir as mybir
from concourse.bass_utils import run_bass_kernel_spmd

F32 = mybir.dt.float32
BF16 = mybir.dt.bfloat16
I32 = mybir.dt.int32
ALU = mybir.AluOpType
AF = mybir.ActivationFunctionType
AX = mybir.AxisListType
ENGS = ("pe", "act", "dve", "pool", "sp")
EPS = 1e-6


class _Op:
    __slots__ = ("eng", "fn", "waits", "dma_key", "milestone", "idx", "needed", "xdeps")

    def __init__(self, eng, fn, dma_key):
        self.eng = eng
        self.fn = fn
        self.waits = []
        self.dma_key = dma_key
        self.milestone = None
        self.needed = False
        self.xdeps = None


class Prog:
    def __init__(self, nc):
        self.nc = nc
        self.ops = []
        self.deps = []
        self.last_writer = {}
        self.readers = {}
        self.dma_count = {}
        self.last_dma = {}
        self.last_eng = {}

    def op(self, eng, fn, reads=(), writes=(), dma_key=None):
        o = _Op(eng, fn, dma_key)
        o.idx = len(self.ops)
        deps = set()
        for k in reads:
            w = self.last_writer.get(k)
            if w is not None:
                deps.add(w)
        for k in writes:
            w = self.last_writer.get(k)
            if w is not None:
                deps.add(w)
            deps.update(self.readers.get(k, ()))
        for k in reads:
            self.readers.setdefault(k, []).append(o.idx)
        for k in writes:
            self.last_writer[k] = o.idx
            self.readers[k] = []
        deps.discard(o.idx)
        self.ops.append(o)
        self.deps.append(sorted(deps))
        if dma_key is not None:
            self.dma_count[dma_key] = self.dma_count.get(dma_key, 0) + 1
            o.milestone = ("dma:" + dma_key, 16 * self.dma_count[dma_key])
            self.last_dma[dma_key] = o.idx
        else:
            self.last_eng[eng] = o.idx
        return o.idx

    def barrier(self):
        tgt = list(self.last_eng.values()) + list(self.last_dma.values())
        for e in ENGS:
            o = _Op(e, None, None)
            o.idx = len(self.ops)
            self.ops.append(o)
            self.deps.append(sorted(tgt))
        self.last_writer = {}
        self.readers = {}

    def _skip(self, y, o):
        return y.eng == o.eng and o.dma_key is None and o.fn is not None and y.eng == "pe"

    def finalize(self):
        ops = self.ops
        for i, o in enumerate(ops):
            for d in self.deps[i]:
                y = ops[d]
                if y.dma_key is not None or y.fn is None:
                    continue
                if self._skip(y, o):
                    continue
                y.needed = True
        cnt = {e: 0 for e in ENGS}
        for o in ops:
            if o.dma_key is None and o.needed:
                cnt[o.eng] += 1
                o.milestone = ("eng:" + o.eng, cnt[o.eng])
        dma_seen = {}
        waited = {e: {} for e in ENGS}
        for i, o in enumerate(ops):
            need = {}
            for d in self.deps[i]:
                y = ops[d]
                if y.fn is None:
                    continue
                if y.dma_key is not None:
                    sem, val = "dma:" + y.dma_key, 16 * dma_seen[y.dma_key]
                else:
                    if y.milestone is None or self._skip(y, o):
                        continue
                    sem, val = y.milestone
                need[sem] = max(need.get(sem, 0), val)
            for sem, val in need.items():
                if waited[o.eng].get(sem, 0) >= val:
                    continue
                waited[o.eng][sem] = val
                o.waits.append((sem, val))
            if o.dma_key is not None:
                dma_seen[o.dma_key] = dma_seen.get(o.dma_key, 0) + 1
        self.sem_names = sorted({o.milestone[0] for o in ops if o.milestone is not None})
        self.final = {}
        for o in ops:
            if o.dma_key is not None:
                self.final.setdefault(o.eng, {})["dma:" + o.dma_key] = 16 * self.dma_count[o.dma_key]

    def emit(self):
        nc = self.nc
        self.finalize()
        with ExitStack() as st:
            sems = {n: st.enter_context(nc.semaphore(n.replace(":", "_"))) for n in self.sem_names}
            block = st.enter_context(nc.Block())
            per = {e: [o for o in self.ops if o.eng == e] for e in ENGS}

            def run(name, e):
                for o in per[name]:
                    for sem, val in o.waits:
                        e.wait_ge(sems[sem], val)
                    if o.fn is None:
                        continue
                    ins = o.fn(e)
                    if o.milestone is not None:
                        ins.then_inc(sems[o.milestone[0]], 16 if o.dma_key is not None else 1)
                for sem, val in self.final.get(name, {}).items():
                    e.wait_ge(sems[sem], val)

            @block.tensor
            def _(e):
                run("pe", e)

            @block.scalar
            def _(e):
                run("act", e)

            @block.vector
            def _(e):
                run("dve", e)

            @block.gpsimd
            def _(e):
                run("pool", e)

            @block.sync
            def _(e):
                run("sp", e)


class Cfg:
    def __init__(self, D=2048, S=4096, L=2, QL=512, KVL=256, DE=1024, CAP=768):
        self.D, self.S, self.L, self.QL, self.KVL, self.DE, self.CAP = D, S, L, QL, KVL, DE, CAP
        self.NH, self.HD, self.RD, self.G, self.EPG = 4, 128, 64, 4, 8
        self.E = self.G * self.EPG
        self.KD, self.KQ, self.KK, self.KF = D // 128, QL // 128, KVL // 128, DE // 128
        self.NT, self.NG = S // 128, S // 512
        self.c_fq, self.c_fk, self.c_sq, self.c_sk = 0, 512, 1024, 1536
        self.c_cq = 2048
        self.c_ckv = self.c_cq + QL
        self.c_pu = self.c_ckv + KVL
        self.c_kr = self.c_pu + 512
        self.c_krs = self.c_kr + 64
        self.c_fv = self.c_krs + 64
        self.c_sv = self.c_fv + 512
        self.c_ff = self.c_sv + 512
        self.c_gate = self.c_ff + 4
        self.NP = self.c_gate + 4 * D
        self.NIN = 1536 + 4 + 1536 + QL + KVL + 64 + 512 + 4 * D


_DTSZ = {F32: 4, BF16: 2, I32: 4}


class Arena:
    def __init__(self, t, nwords):
        self.t, self.n, self.off, self.base = t, nwords, 0, 0

    def alloc(self, shape, dt):
        n = int(np.prod(shape))
        words = (n * _DTSZ[dt] + 3) // 4
        assert self.off + words <= self.n, ("SBUF arena overflow", self.off, words, self.n)
        v = self.t[:, self.off:self.off + words]
        self.off += words
        if dt != F32:
            v = v.bitcast(dt)
            v = v[:, 0:n]
        if len(shape) == 2:
            v = v.rearrange("p (a b) -> p a b", a=shape[0])
        elif len(shape) == 3:
            v = v.rearrange("p (a b c) -> p a b c", a=shape[0], b=shape[1])
        return v

    def mark_persistent(self):
        self.base = self.off

    def reset(self):
        self.off = self.base


def build_program(cfg, debug=False):
    c = cfg
    D, S, L, KD, KQ, KK, KF, NT, NG, E, CAP, NH = c.D, c.S, c.L, c.KD, c.KQ, c.KK, c.KF, c.NT, c.NG, c.E, c.CAP, c.NH
    QL, KVL, DE = c.QL, c.KVL, c.DE
    nc = bass.Bass("TRN2", target_bir_lowering=False)
    P = Prog(nc)

    def din(name, shape, dt=F32):
        return nc.dram_tensor(name, list(shape), dt, kind="ExternalInput").ap()

    def dscr(name, shape, dt):
        return nc.dram_tensor(name, list(shape), dt, kind=("ExternalOutput" if debug else "Internal")).ap()

    x_in = din("x", [S, D])
    cT_in = din("cT", [128, KD])
    w_mod = din("w_mod", [L, D, 6 * D])
    b_mod_rep = din("b_mod_rep", [L, 128, 6 * D])
    g1_rep = din("g1_rep", [L, 128, D])
    g2_rep = din("g2_rep", [L, 128, D])
    gf_rep = din("gf_rep", [128, D])
    w_in = din("w_in_p", [L, D, c.NP])
    bf_rep = din("bf_rep", [L, 128, 4])
    gq_in = din("gq", [L, 128, KQ])
    w_uq = din("w_uq_p", [L, QL, 1024])
    gkv_in = din("gkv", [L, 128, KK])
    w_ukv = din("w_ukv_p", [L, KVL, 1024])
    w_pool = din("w_pool", [L, 4, 128, 128])
    pscale_in = din("pscale", [L, 128, 4])
    w_branch = din("w_branch", [L, 4, 512, D])
    w_out = din("w_out", [L, D, D])
    w_r = din("w_r", [L, D, 36])
    b_r_rep = din("b_r_rep", [L, 128, 36])
    w_gate = din("w_gate", [L, E, D, DE])
    w_up = din("w_up", [L, E, D, DE])
    w_down = din("w_down", [L, E, DE, D])
    consts_in = din("consts", [128, 7, 128])
    rope_in = din("rope", [2, 64, S])
    invcnt_in = din("invcnt", [128, 4, S])
    ecap_in = din("ecap", [128, 32])
    out = nc.dram_tensor("out", [S, D], F32, kind="ExternalOutput").ap()
    cnt_out = nc.dram_tensor("cnt", [L, 128, 32], F32, kind="ExternalOutput").ap()

    xres = dscr("xres", [S, D], F32)
    xmid = dscr("xmid", [S, D], F32)
    modr = dscr("modr", [128, 6 * D], F32)
    hT = dscr("hT", [D, S], BF16)
    h2 = dscr("h2", [S, D], BF16)
    qT = dscr("qT", [3, 512, S], BF16)
    kT = dscr("kT", [3, 512, S], BF16)
    qrT = dscr("qrT", [NH * 64, S], BF16)
    krT = dscr("krT", [64, S], BF16)
    vv = dscr("vv", [3, S, 512], BF16)
    yT = dscr("yT", [4 * 512, S], BF16)
    x_buf = dscr("x_buf", [E * CAP, D], BF16)
    w_in_b = [dscr(f"w_in_b{i}", [D, c.NP], BF16) for i in range(2)]
    w_br_b = [dscr(f"w_br_b{i}", [16 * 128, D], BF16) for i in range(2)]
    w_out_b = [dscr(f"w_out_b{i}", [D, D], BF16) for i in range(2)]
    y_buf = dscr("y_buf", [E * CAP, D], F32)

    with ExitStack() as st:
        ARW = 51200
        arena_t = st.enter_context(nc.sbuf_tensor("arena", [128, ARW], F32))
        A = Arena(arena_t, ARW)
        NPS = 7
        PS = [st.enter_context(nc.psum_tensor(f"ps{i}", [128, 512], F32)) for i in range(NPS)]
        PSB = st.enter_context(nc.psum_tensor("psb", [128, 1024], BF16))
        PSK = [f"ps{i}" for i in range(NPS)]

        cst = A.alloc([7, 128], F32)
        cstb = A.alloc([7, 128], BF16)
        identf, onesf, sel127f, leF = cst[:, 0, :], cst[:, 4, :], cst[:, 5, :], cst[:, 1, :]
        identb, leb, ltb, geb, onesb = cstb[:, 0, :], cstb[:, 1, :], cstb[:, 2, :], cstb[:, 3, :], cstb[:, 4, :]
        LF = A.alloc([NT, 4], F32)
        FC = A.alloc([NT, 4], F32)
        SLOT = A.alloc([NT, 2], I32)
        WTS = A.alloc([NT, 2], F32)
        ecap = A.alloc([32], F32)
        carry = A.alloc([32], F32)
        A.mark_persistent()

        dmaq = {"n": 0}

        def sp_dma(out_ap, in_ap, reads=(), writes=(), key=None):
            P.op("sp", lambda e, o=out_ap, i=in_ap: e.dma_start(out=o, in_=i), reads=reads, writes=writes, dma_key=key)

        def pool_dma(out_ap, in_ap, reads=(), writes=(), key=None):
            P.op("pool", lambda e, o=out_ap, i=in_ap: e.dma_start(out=o, in_=i), reads=reads, writes=writes, dma_key=key)

        def mm(ps, lhsT, rhs, start, stop, reads, writes, skip=False):
            P.op("pe", lambda e, a=ps, b=lhsT, cc=rhs, s0=start, s1=stop, sk=skip: e.matmul(a, lhsT=b, rhs=cc, start=s0, stop=s1, skip_group_check=sk),
                 reads=reads, writes=writes)

        def tt(eng, out_ap, in0, in1, op, reads, writes):
            P.op(eng, lambda e, o=out_ap, a=in0, b=in1, p=op: e.tensor_tensor(out=o, in0=a, in1=b, op=p), reads=reads, writes=writes)

        def ts(eng, out_ap, in0, s1, s2, op0, op1, reads, writes):
            P.op(eng, lambda e, o=out_ap, a=in0, x1=s1, x2=s2, p0=op0, p1=op1: e.tensor_scalar(out=o, in0=a, scalar1=x1, scalar2=x2, op0=p0, op1=p1),
                 reads=reads, writes=writes)

        def stt(eng, out_ap, in0, scalar, in1, op0, op1, reads, writes):
            P.op(eng, lambda e, o=out_ap, a=in0, s=scalar, b=in1, p0=op0, p1=op1: e.scalar_tensor_tensor(out=o, in0=a, scalar=s, in1=b, op0=p0, op1=p1),
                 reads=reads, writes=writes)

        def act(out_ap, in_ap, func, reads, writes, **kw):
            P.op("act", lambda e, o=out_ap, i=in_ap, f=func, k=kw: e.activation(out=o, in_=i, func=f, **k), reads=reads, writes=writes)

        def cp(eng, out_ap, in_ap, reads, writes):
            if eng == "act":
                P.op("act", lambda e, o=out_ap, i=in_ap: e.copy(out=o, in_=i), reads=reads, writes=writes)
            else:
                P.op(eng, lambda e, o=out_ap, i=in_ap: e.tensor_copy(out=o, in_=i), reads=reads, writes=writes)

        def memset(eng, ap, val, writes):
            P.op(eng, lambda e, a=ap, v=val: e.memset(a, v), writes=writes)

        def rstd_from_ssq(ssq, n, rkeys):
            ts("dve", ssq, ssq, 1.0 / n, EPS, ALU.mult, ALU.add, rkeys, rkeys)
            P.op("act", lambda e, a=ssq: e.sqrt(out=a, in_=a), reads=rkeys, writes=rkeys)
            P.op("dve", lambda e, a=ssq: e.reciprocal(out=a, in_=a), reads=rkeys, writes=rkeys)

        def convert_weights(ll, stg):
            jobs = []
            src = w_in[ll].rearrange("(k p) n -> p k n", p=128)
            dst = w_in_b[ll % 2].rearrange("(k p) n -> p k n", p=128)
            for c0 in range(0, c.NP, 512):
                w = min(512, c.NP - c0)
                jobs.append((src[:, :, c0:c0 + w], dst[:, :, c0:c0 + w], KD, w))
            src = w_branch[ll].rearrange("n (h p) d -> p (n h) d", p=128)
            dst = w_br_b[ll % 2].rearrange("(k p) d -> p k d", p=128)
            for c0 in range(0, D, 512):
                w = min(512, D - c0)
                jobs.append((src[:, :, c0:c0 + w], dst[:, :, c0:c0 + w], 16, w))
            src = w_out[ll].rearrange("(k p) n -> p k n", p=128)
            dst = w_out_b[ll % 2].rearrange("(k p) n -> p k n", p=128)
            for c0 in range(0, D, 512):
                w = min(512, D - c0)
                jobs.append((src[:, :, c0:c0 + w], dst[:, :, c0:c0 + w], KD, w))
            for i, (sa_, da_, nk, w) in enumerate(jobs):
                bi = i % 2
                pool_dma(stg[bi][:, 0:nk, 0:w], sa_, writes=[f"stg{bi}"], key=f"stg{bi}")
                pool_dma(da_, stg[bi][:, 0:nk, 0:w], reads=[f"stg{bi}"], key=f"stgo{bi}")

        regs = {}

        def _mk_bk(e):
            regs["bk"] = e.alloc_register("bk")
            return e.reg_mov(regs["bk"], E * CAP - 1)

        P.op("pool", _mk_bk)
        sp_dma(cst, consts_in[:, :, :], writes=["cst"], key="c0")
        sp_dma(ecap, ecap_in[:, :], writes=["ecap"], key="c1")
        cp("dve", cstb, cst, ["cst"], ["cstb"])
        A.reset()
        stg0 = [A.alloc([max(KD, 16), 512], BF16) for _ in range(2)]
        convert_weights(0, stg0)
        P.barrier()

        for l in range(L):
            last = (l == L - 1)
            xsrc = x_in if l == 0 else xres
            A.reset()
            cTt = A.alloc([KD], F32)
            crep = A.alloc([KD, 128], F32)
            modrep = A.alloc([6 * D], F32)
            gtmp = A.alloc([D], F32)
            wm = [A.alloc([KD, 512], F32) for _ in range(2)]
            sp_dma(cTt, cT_in[:, :], writes=["cTt"], key="m0")
            sp_dma(modrep, b_mod_rep[l], writes=["modrep"], key="m1")
            act(cTt, cTt, AF.Silu, ["cTt"], ["cTt"])
            for k in range(KD):
                cp("dve", crep[:, k, :], cTt[:, k:k + 1].to_broadcast([128, 128]), ["cTt"], ["crep"])
            wmv = w_mod[l].rearrange("(k p) n -> p k n", p=128)
            NCH = 6 * D // 512
            for n in range(NCH):
                b = n % 2
                sp_dma(wm[b], wmv[:, :, n * 512:(n + 1) * 512], writes=[f"wm{b}"], key=f"wm{b}")
                ps = n % 2
                for k in range(KD):
                    mm(PS[ps][:, :], crep[:, k, :], wm[b][:, k, :], k == 0, k == KD - 1, ["crep", f"wm{b}"], [PSK[ps]])
                sl = slice(n * 512, (n + 1) * 512)
                tt("dve", modrep[:, sl], modrep[:, sl], PS[ps][:, :], ALU.add, ["modrep", PSK[ps]], ["modrep"])
            for (gsrc, off) in ((g1_rep, D), (g2_rep, 4 * D)):
                sp_dma(gtmp, gsrc[l], writes=["gtmp"], key="m2")
                stt("dve", modrep[:, off:off + D], modrep[:, off:off + D], 1.0, gtmp, ALU.add, ALU.mult, ["modrep", "gtmp"], ["modrep"])
            sp_dma(modr[:, :], modrep, reads=["modrep"], key="m3")
            P.barrier()

            A.reset()
            memset("dve", LF, 0.0, ["LF"])
            gm1 = A.alloc([D], F32)
            sh1 = A.alloc([D], F32)
            xt = [A.alloc([D], F32) for _ in range(2)]
            tmpf = A.alloc([D], F32)
            hb = A.alloc([D], BF16)
            hTg = A.alloc([KD, 512], BF16)
            slab = [A.alloc([KD, 640], BF16) for _ in range(2)]
            ob = [A.alloc([512], BF16) for _ in range(4)]
            cq = A.alloc([KQ, 512], F32)
            ckv = A.alloc([KK, 512], F32)
            sqb = A.alloc([512], F32)
            rstd = A.alloc([512], F32)
            cqn = A.alloc([KQ, 512], BF16)
            ckvn = A.alloc([KK, 512], BF16)
            wuq = A.alloc([KQ, 1024], BF16)
            wukv = A.alloc([KK, 1024], BF16)
            wpl = A.alloc([4, 128], BF16)
            gq = A.alloc([KQ], F32)
            gkv = A.alloc([KK], F32)
            psc = A.alloc([4], F32)
            bfr = A.alloc([4], F32)
            halo = A.alloc([4, 16], F32)
            pu = A.alloc([528], F32)
            sa = A.alloc([528], F32)
            sb_ = A.alloc([528], F32)
            icn = A.alloc([512], F32)
            pooled = A.alloc([512], BF16)
            rcos = A.alloc([512], F32)
            rsin = A.alloc([512], F32)
            rt1 = A.alloc([512], F32)
            rt2 = A.alloc([512], F32)
            ssq = A.alloc([4], F32)
            fz = A.alloc([16], F32)

            sp_dma(gm1, modr[:, D:2 * D], writes=["gm1"], key="a0")
            sp_dma(sh1, modr[:, 0:D], writes=["sh1"], key="a0")
            sp_dma(gq, gq_in[l], writes=["gq"], key="a0")
            sp_dma(gkv, gkv_in[l], writes=["gkv"], key="a0")
            sp_dma(psc, pscale_in[l], writes=["psc"], key="a0")
            sp_dma(bfr, bf_rep[l], writes=["bfr"], key="a0")
            pool_dma(wuq, w_uq[l].rearrange("(k p) n -> p k n", p=128), writes=["wuq"], key="a1")
            pool_dma(wukv, w_ukv[l].rearrange("(k p) n -> p k n", p=128), writes=["wukv"], key="a1")
            pool_dma(wpl, w_pool[l].rearrange("g c d -> c g d"), writes=["wpl"], key="a1")
            memset("dve", halo, 0.0, ["halo"])
            winv = w_in_b[l % 2].rearrange("(k p) n -> p k n", p=128)
            hTv = hT.rearrange("(k p) s -> p k s", p=128)
            psrot = {"i": 0}

            def nextps():
                psrot["i"] = (psrot["i"] + 1) % 5
                return psrot["i"]

            obrot = {"i": 0}

            def store_fm(psi, dst_ap, eng_i, npart=128):
                i = obrot["i"] = (obrot["i"] + 1) % 4
                cp("act" if eng_i % 2 == 0 else "dve", ob[i][0:npart, :], PS[psi][0:npart, :], [PSK[psi]], [f"ob{i}"])
                sp_dma(dst_ap, ob[i][0:npart, :], reads=[f"ob{i}"], key=f"ob{i}")

            slrot = {"i": 0}

            def load_slab(c0, w):
                i = slrot["i"] = (slrot["i"] + 1) % 2
                pool_dma(slab[i][:, :, 0:w], winv[:, :, c0:c0 + w], writes=[f"slab{i}"], key=f"slab{i}")
                return i

            for g in range(NG):
                ts0 = g * 512
                for t4 in range(4):
                    tix = g * 4 + t4
                    b = tix % 2
                    sp_dma(xt[b], xsrc[tix * 128:(tix + 1) * 128, :], writes=[f"xt{b}"], key=f"xt{b}")
                    memset("dve", ssq[:, 0:1], 0.0, ["ssq"])
                    act(tmpf, xt[b], AF.Square, [f"xt{b}"], ["tmpf", "ssq"], accum_out=ssq[:, 0:1])
                    rstd_from_ssq(ssq[:, 0:1], D, ["ssq"])
                    stt("dve", tmpf, xt[b], ssq[:, 0:1], gm1, ALU.mult, ALU.mult, [f"xt{b}", "ssq", "gm1"], ["tmpf"])
                    tt("dve", hb, tmpf, sh1, ALU.add, ["tmpf", "sh1"], ["hb"])
                    for k4 in range(0, KD, 8):
                        nk = min(8, KD - k4)
                        for kk in range(nk):
                            k = k4 + kk
                            P.op("pe", lambda e, o=PSB[:, kk * 128:(kk + 1) * 128], i=hb[:, k * 128:(k + 1) * 128]: e.transpose(out=o, in_=i, identity=identb),
                                 reads=["hb", "cstb"], writes=["psb"])
                        cp("act" if (k4 // 8) % 2 == 0 else "dve", hTg[:, k4:k4 + nk, t4 * 128:(t4 + 1) * 128],
                           PSB[:, 0:nk * 128].rearrange("p (a b) -> p a b", a=nk), ["psb"], ["hTg"])
                sp_dma(hTv[:, :, ts0:ts0 + 512], hTg, reads=["hTg"], key="hTst")
                ei = 0
                for (c0, dst_t, dst_b) in ((c.c_fq, qT, 0), (c.c_fk, kT, 0), (c.c_sq, qT, 1), (c.c_sk, kT, 1)):
                    si = load_slab(c0, 512)
                    for j in range(4):
                        psi = nextps()
                        for k in range(KD):
                            mm(PS[psi][:, :], slab[si][:, k, j * 128:(j + 1) * 128], hTg[:, k, :], k == 0, k == KD - 1, [f"slab{si}", "hTg"], [PSK[psi]])
                        store_fm(psi, dst_t[dst_b, j * 128:(j + 1) * 128, ts0:ts0 + 512], ei)
                        ei += 1
                for (c0, nchunk, dstt, dkey) in ((c.c_cq, KQ, cq, "cq"), (c.c_ckv, KK, ckv, "ckv")):
                    for j0 in range(0, nchunk, 4):
                        nj = min(4, nchunk - j0)
                        si = load_slab(c0 + j0 * 128, nj * 128)
                        for j in range(nj):
                            psi = nextps()
                            for k in range(KD):
                                mm(PS[psi][:, :], slab[si][:, k, j * 128:(j + 1) * 128], hTg[:, k, :], k == 0, k == KD - 1, [f"slab{si}", "hTg"], [PSK[psi]])
                            cp("act" if j % 2 == 0 else "dve", dstt[:, j0 + j, :], PS[psi][:, :], [PSK[psi]], [dkey])
                si = load_slab(c.c_pu, 512 + 128)
                sp_dma(rcos[0:64, :], rope_in[0, :, ts0:ts0 + 512], writes=["rcos"], key="rope")
                sp_dma(rsin[0:64, :], rope_in[1, :, ts0:ts0 + 512], writes=["rsin"], key="rope")
                for gi in range(4):
                    psi = nextps()
                    for k in range(KD):
                        mm(PS[psi][:, :], slab[si][:, k, gi * 128:(gi + 1) * 128], hTg[:, k, :], k == 0, k == KD - 1, [f"slab{si}", "hTg"], [PSK[psi]])
                    cp("dve", pu[:, 0:16], halo[:, gi, :], ["halo"], ["pu"])
                    cp("act", pu[:, 16:528], PS[psi][:, :], [PSK[psi]], ["pu"])
                    cp("dve", halo[:, gi, :], pu[:, 512:528], ["pu"], ["halo"])
                    w = 2 << gi
                    sp_dma(icn, invcnt_in[:, gi, ts0:ts0 + 512], writes=["icn"], key="icn")
                    src, srck = pu, "pu"
                    step = 1
                    bufs = [(sa, "sa"), (sb_, "sb_")]
                    bi = 0
                    while step < w:
                        dstb, dk = bufs[bi]
                        lo = 2 * step - 1
                        tt("dve", dstb[:, lo:528], src[:, lo:528], src[:, lo - step:528 - step], ALU.add, [srck], [dk])
                        src, srck = dstb, dk
                        bi ^= 1
                        step *= 2
                    tt("dve", rt1, src[:, 16:528], icn, ALU.mult, [srck, "icn"], ["rt1"])
                    tt("dve", pooled, rt1, pu[:, 16:528], ALU.subtract, ["rt1", "pu"], ["pooled"])
                    psj = nextps()
                    mm(PS[psj][:, :], wpl[:, gi, :], pooled, True, True, ["wpl", "pooled"], [PSK[psj]])
                    i = obrot["i"] = (obrot["i"] + 1) % 4
                    ts("dve", ob[i], PS[psj][:, :], psc[:, gi:gi + 1], 0.0, ALU.mult, ALU.add, [PSK[psj], "psc"], [f"ob{i}"])
                    sp_dma(yT[3 * 512 + gi * 128:3 * 512 + (gi + 1) * 128, ts0:ts0 + 512], ob[i], reads=[f"ob{i}"], key=f"ob{i}")
                psa, psb2 = nextps(), nextps()
                for (psi, cc0) in ((psa, 512), (psb2, 576)):
                    for k in range(KD):
                        mm(PS[psi][0:64, :], slab[si][:, k, cc0:cc0 + 64], hTg[:, k, :], k == 0, k == KD - 1, [f"slab{si}", "hTg"], [PSK[psi]])
                tt("dve", rt1[0:64, :], PS[psa][0:64, :], rcos[0:64, :], ALU.mult, [PSK[psa], "rcos"], ["rt1"])
                tt("dve", rt2[0:64, :], PS[psb2][0:64, :], rsin[0:64, :], ALU.mult, [PSK[psb2], "rsin"], ["rt2"])
                i = obrot["i"] = (obrot["i"] + 1) % 4
                tt("dve", ob[i][0:64, :], rt1[0:64, :], rt2[0:64, :], ALU.add, ["rt1", "rt2"], [f"ob{i}"])
                sp_dma(krT[:, ts0:ts0 + 512], ob[i][0:64, :], reads=[f"ob{i}"], key=f"ob{i}")
                for (c0, bidx) in ((c.c_fv, 0), (c.c_sv, 1)):
                    si = load_slab(c0, 512)
                    for t4 in range(4):
                        psi = nextps()
                        for k in range(KD):
                            mm(PS[psi][:, :], hTg[:, k, t4 * 128:(t4 + 1) * 128], slab[si][:, k, 0:512], k == 0, k == KD - 1, [f"slab{si}", "hTg"], [PSK[psi]])
                        store_fm(psi, vv[bidx, ts0 + t4 * 128:ts0 + (t4 + 1) * 128, :], t4)
                si = load_slab(c.c_ff, 4)
                psi = nextps()
                for t4 in range(4):
                    for k in range(KD):
                        mm(PS[psi][:, t4 * 4:(t4 + 1) * 4], hTg[:, k, t4 * 128:(t4 + 1) * 128], slab[si][:, k, 0:4], k == 0, k == KD - 1,
                           [f"slab{si}", "hTg"], [PSK[psi]], skip=True)
                tt("dve", fz.rearrange("p (a b) -> p a b", a=4), PS[psi][:, 0:16].rearrange("p (a b) -> p a b", a=4),
                   bfr.unsqueeze(1).to_broadcast([128, 4, 4]), ALU.add, [PSK[psi], "bfr"], ["fz"])
                act(fz, fz, AF.Exp, ["fz"], ["fz"], scale=-1.0)
                act(fz, fz, AF.Ln, ["fz"], ["fz"], bias=1.0)
                ts("dve", LF[:, g * 4:(g + 1) * 4, :], fz.rearrange("p (a b) -> p a b", a=4), -1.0, 0.0, ALU.mult, ALU.add, ["fz"], ["LF"])
                for (src_t, skey, nchunk, nfeat, gvec, gkey, dst_n, dkey) in ((cq, "cq", KQ, QL, gq, "gq", cqn, "cqn"), (ckv, "ckv", KK, KVL, gkv, "gkv", ckvn, "ckvn")):
                    psi = nextps()
                    for k in range(nchunk):
                        act(sqb, src_t[:, k, :], AF.Square, [skey], ["sqb"])
                        mm(PS[psi][:, :], onesf, sqb, k == 0, k == nchunk - 1, ["cst", "sqb"], [PSK[psi]])
                    cp("dve", rstd, PS[psi][:, :], [PSK[psi]], ["rstd"])
                    rstd_from_ssq(rstd, nfeat, ["rstd"])
                    for k in range(nchunk):
                        stt("dve", dst_n[:, k, :], src_t[:, k, :], gvec[:, k:k + 1], rstd, ALU.mult, ALU.mult, [skey, gkey, "rstd"], [dkey])
                for h in range(NH):
                    psi = nextps()
                    for k in range(KQ):
                        mm(PS[psi][:, :], wuq[:, k, h * 128:(h + 1) * 128], cqn[:, k, :], k == 0, k == KQ - 1, ["wuq", "cqn"], [PSK[psi]])
                    store_fm(psi, qT[2, h * 128:(h + 1) * 128, ts0:ts0 + 512], h)
                    psa, psb2 = nextps(), nextps()
                    for (pq, cc0) in ((psa, 512 + h * 128), (psb2, 512 + h * 128 + 64)):
                        for k in range(KQ):
                            mm(PS[pq][0:64, :], wuq[:, k, cc0:cc0 + 64], cqn[:, k, :], k == 0, k == KQ - 1, ["wuq", "cqn"], [PSK[pq]])
                    tt("dve", rt1[0:64, :], PS[psa][0:64, :], rcos[0:64, :], ALU.mult, [PSK[psa], "rcos"], ["rt1"])
                    tt("dve", rt2[0:64, :], PS[psb2][0:64, :], rsin[0:64, :], ALU.mult, [PSK[psb2], "rsin"], ["rt2"])
                    i = obrot["i"] = (obrot["i"] + 1) % 4
                    tt("dve", ob[i][0:64, :], rt1[0:64, :], rt2[0:64, :], ALU.add, ["rt1", "rt2"], [f"ob{i}"])
                    sp_dma(qrT[h * 64:(h + 1) * 64, ts0:ts0 + 512], ob[i][0:64, :], reads=[f"ob{i}"], key=f"ob{i}")
                for h in range(NH):
                    psi = nextps()
                    for k in range(KK):
                        mm(PS[psi][:, :], wukv[:, k, h * 128:(h + 1) * 128], ckvn[:, k, :], k == 0, k == KK - 1, ["wukv", "ckvn"], [PSK[psi]])
                    store_fm(psi, kT[2, h * 128:(h + 1) * 128, ts0:ts0 + 512], h)
                for t4 in range(4):
                    psi = nextps()
                    for k in range(KK):
                        mm(PS[psi][:, :], ckvn[:, k, t4 * 128:(t4 + 1) * 128], wukv[:, k, 512:1024], k == 0, k == KK - 1, ["wukv", "ckvn"], [PSK[psi]])
                    store_fm(psi, vv[2, ts0 + t4 * 128:ts0 + (t4 + 1) * 128, :], t4)
            for i in range(NT):
                psi = nextps()
                for j in range(i + 1):
                    lhs = leF if j == i else onesf
                    mm(PS[psi][:, 0:4], lhs, LF[:, j, :], j == 0, j == i, ["cst", "LF"], [PSK[psi]])
                cp("dve", FC[:, i, :], PS[psi][:, 0:4], [PSK[psi]], ["FC"])
            P.barrier()

            A.reset()
            Fd = A.alloc([NH, NT * NT], F32)
            fend = A.alloc([NT * 4], F32)
            KTt = [A.alloc([S], BF16) for _ in range(2)]
            QTt = [A.alloc([S], BF16) for _ in range(2)]
            Vt = [A.alloc([NT, 128], BF16) for _ in range(2)]
            KRt = A.alloc([S], BF16)
            QRt = [A.alloc([S], BF16) for _ in range(2)]
            PT = [A.alloc([512], BF16) for _ in range(2)]
            Eb = [A.alloc([512], F32) for _ in range(2)]
            tmpb = [A.alloc([512], F32) for _ in range(2)]
            Lb = [A.alloc([512], BF16) for _ in range(2)]
            Srun = A.alloc([512], F32)
            rd = A.alloc([512], F32)
            ytb = [A.alloc([512], BF16) for _ in range(2)]
            if l + 1 < L:
                stgn = [A.alloc([max(KD, 16), 512], BF16) for _ in range(2)]
                convert_weights(l + 1, stgn)
            mm(PS[0][:, 0:NT * 4], sel127f, FC.rearrange("p a b -> p (a b)"), True, True, ["cst", "FC"], [PSK[0]])
            cp("dve", fend, PS[0][:, 0:NT * 4], [PSK[0]], ["fend"])
            fend3 = fend.rearrange("p (a b) -> p a b", b=4)
            for h in range(NH):
                for kb in range(NT):
                    ts("dve", Fd[:, h, kb * NT:(kb + 1) * NT], fend3[:, :, h], FC[:, kb, h:h + 1], 0.0, ALU.subtract, ALU.add, ["fend", "FC"], ["Fd"])
            sp_dma(KRt[0:64, :], krT[:, :], writes=["KRt"], key="att_kr")
            hrot = 0
            for br in range(3):
                scale = (128.0 if br < 2 else 192.0) ** -0.5
                for h in range(NH):
                    b = hrot % 2
                    hrot += 1
                    sp_dma(KTt[b], kT[br, h * 128:(h + 1) * 128, :], writes=[f"KT{b}"], key=f"KT{b}")
                    sp_dma(QTt[b], qT[br, h * 128:(h + 1) * 128, :], writes=[f"QT{b}"], key=f"QT{b}")
                    vsrc = vv[br, :, h * 128:(h + 1) * 128].rearrange("(t p) d -> p t d", p=128)
                    for v0 in range(0, NT, 8):
                        sp_dma(Vt[b][:, v0:v0 + 8, :], vsrc[:, v0:v0 + 8, :], writes=[f"V{b}"], key=f"V{b}")
                    if br == 2:
                        sp_dma(QRt[b][0:64, :], qrT[h * 64:(h + 1) * 64, :], writes=[f"QR{b}"], key=f"QR{b}")
                    inkeys = [f"KT{b}", f"QT{b}"] + (["KRt", f"QR{b}"] if br == 2 else [])
                    for g in range(NG):
                        q0 = g * 512
                        nkb = 4 * g + 4
                        order = list(range(nkb)) if br != 1 else list(range(nkb - 1, -1, -1))
                        if br == 1:
                            memset("dve", Srun, 0.0, ["Srun"])
                        ya, da = ((2, 3) if (br == 1 or g % 2 == 0) else (4, 5))

                        def geom(oi):
                            kb = order[oi]
                            c0 = 0 if kb < 4 * g else 128 * (kb - 4 * g)
                            return kb, c0, kb >= 4 * g

                        def qk(oi):
                            kb, c0, _ = geom(oi)
                            pss = oi % 2
                            mm(PS[pss][:, c0:512], KTt[b][:, kb * 128:(kb + 1) * 128], QTt[b][:, q0 + c0:q0 + 512], True, br != 2, inkeys, [PSK[pss]])
                            if br == 2:
                                mm(PS[pss][:, c0:512], KRt[0:64, kb * 128:(kb + 1) * 128], QRt[b][0:64, q0 + c0:q0 + 512], False, True, inkeys, [PSK[pss]])

                        def pv(oi):
                            kb, c0, _ = geom(oi)
                            pt, ptk = PT[oi % 2], f"PT{oi % 2}"
                            mm(PS[ya][:, c0:512], Vt[b][:, kb, :], pt[:, c0:512], oi == 0, oi == nkb - 1, [f"V{b}", ptk], [PSK[ya]], skip=True)
                            if br != 1:
                                mm(PS[da][:, c0:512], onesb, pt[:, c0:512], oi == 0, oi == nkb - 1, ["cstb", ptk], [PSK[da]], skip=True)

                        if br != 1:
                            qk(0)
                            for oi in range(nkb):
                                kb, c0, diag = geom(oi)
                                pss = oi % 2
                                pt, ptk = PT[oi % 2], f"PT{oi % 2}"
                                if oi + 1 < nkb:
                                    qk(oi + 1)
                                if br == 0:
                                    for j2 in range(2):
                                        lo, hi = max(c0, 256 * j2), 256 * (j2 + 1)
                                        if lo >= hi:
                                            continue
                                        qb = 4 * g + 2 * j2
                                        act(pt[:, lo:hi], PS[pss][:, lo:hi], AF.Exp, [PSK[pss], "Fd"], [ptk],
                                            scale=scale, bias=Fd[:, h, kb * NT + qb:kb * NT + qb + 1])
                                else:
                                    act(pt[:, c0:512], PS[pss][:, c0:512], AF.Exp, [PSK[pss]], [ptk], scale=scale)
                                if diag:
                                    tt("dve", pt[:, c0:c0 + 128], pt[:, c0:c0 + 128], leb, ALU.mult, [ptk, "cstb"], [ptk])
                                pv(oi)
                        else:
                            Rb, Cb = (4, 6), (5, 3)

                            def stage1(oi):
                                kb, c0, diag = geom(oi)
                                pss = oi % 2
                                eb, ebk = Eb[oi % 2], f"Eb{oi % 2}"
                                lb, lbk = Lb[oi % 2], f"Lb{oi % 2}"
                                act(eb[:, c0:512], PS[pss][:, c0:512], AF.Exp, [PSK[pss]], [ebk], scale=scale)
                                act(lb[:, c0:512], eb[:, c0:512], AF.Ln, [ebk], [lbk], bias=1.0)
                                if diag:
                                    tt("pool", lb[:, c0:c0 + 128], lb[:, c0:c0 + 128], ltb, ALU.mult, [lbk, "cstb"], [lbk])
                                r_, c_ = Rb[oi % 2], Cb[oi % 2]
                                mm(PS[r_][:, c0:512], geb, lb[:, c0:512], True, True, ["cstb", lbk], [PSK[r_]])
                                mm(PS[c_][:, c0:512], onesb, lb[:, c0:512], True, True, ["cstb", lbk], [PSK[c_]])

                            def stage2(oi):
                                kb, c0, diag = geom(oi)
                                pt, ptk = PT[oi % 2], f"PT{oi % 2}"
                                eb, ebk = Eb[oi % 2], f"Eb{oi % 2}"
                                tb, tbk = tmpb[oi % 2], f"tmpb{oi % 2}"
                                r_, c_ = Rb[oi % 2], Cb[oi % 2]
                                tt("dve", tb[:, c0:512], PS[r_][:, c0:512], Srun[:, c0:512], ALU.add, [PSK[r_], "Srun"], [tbk])
                                act(tb[:, c0:512], tb[:, c0:512], AF.Exp, [tbk], [tbk], scale=-1.0)
                                tt("dve", pt[:, c0:512], eb[:, c0:512], tb[:, c0:512], ALU.mult, [ebk, tbk], [ptk])
                                if diag:
                                    tt("pool", pt[:, c0:c0 + 128], pt[:, c0:c0 + 128], ltb, ALU.mult, [ptk, "cstb"], [ptk])
                                tt("dve", Srun[:, c0:512], Srun[:, c0:512], PS[c_][:, c0:512], ALU.add, ["Srun", PSK[c_]], ["Srun"])

                            qk(0)
                            if nkb > 1:
                                qk(1)
                            stage1(0)
                            for oi in range(nkb):
                                if oi + 2 < nkb:
                                    qk(oi + 2)
                                if oi + 1 < nkb:
                                    stage1(oi + 1)
                                if oi > 0:
                                    pv(oi - 1)
                                stage2(oi)
                            pv(nkb - 1)
                        yi = (g + hrot) % 2
                        if br != 1:
                            P.op("dve", lambda e, o=rd, d_=da: e.reciprocal(out=o, in_=PS[d_][:, :]), reads=[PSK[da]], writes=["rd"])
                            tt("dve", ytb[yi], PS[ya][:, :], rd, ALU.mult, [PSK[ya], "rd"], [f"ytb{yi}"])
                        else:
                            cp("act", ytb[yi], PS[ya][:, :], [PSK[ya]], [f"ytb{yi}"])
                        sp_dma(yT[br * 512 + h * 128:br * 512 + (h + 1) * 128, q0:q0 + 512], ytb[yi], reads=[f"ytb{yi}"], key=f"ytb{yi}")
            P.barrier()

            A.reset()
            gate1 = A.alloc([D], F32)
            gm2 = A.alloc([D], F32)
            sh2 = A.alloc([D], F32)
            RA = A.alloc([KD, 512], BF16)
            RB = A.alloc([16, 512], BF16)
            gsl = [A.alloc([KD, 256], BF16) for _ in range(3)]
            bsl = [A.alloc([4, 256], BF16) for _ in range(3)]
            sg = [A.alloc([512], F32) for _ in range(2)]
            acc = [A.alloc([512], F32) for _ in range(2)]
            mtmp = A.alloc([512], F32)
            mrg = A.alloc([KD, 512], BF16)
            wos = [A.alloc([KD, 256], BF16) for _ in range(2)]
            xtmp = A.alloc([256], F32)
            h2f = A.alloc([D], F32)
            h2b = A.alloc([D], BF16)
            h2T = A.alloc([KD, 128], F32)
            wr = A.alloc([KD, 36], F32)
            brr = A.alloc([36], F32)
            lg = A.alloc([36], F32)
            sm = A.alloc([16], F32)
            esel = A.alloc([8], F32)
            esel2 = A.alloc([8], F32)
            oh = A.alloc([2, 8], F32)
            goh = A.alloc([4], F32)
            gex = A.alloc([4], F32)
            A1 = A.alloc([2, 32], F32)
            Ab = A.alloc([32], BF16)
            posC = A.alloc([32], F32)
            ptmp = A.alloc([32], F32)
            slf = A.alloc([2], F32)
            zt = A.alloc([D], BF16)
            xtl = []
            for i in range(4):
                reg = RA if i < 2 else RB
                flat = reg.rearrange("p a b -> p (a b)").bitcast(F32)
                xtl.append(flat[:, (i % 2) * D:(i % 2 + 1) * D])
            xkeys = ["RA", "RA", "RB", "RB"]
            assert KD * 256 >= 2 * D and 16 * 256 >= 2 * D, "alias regions too small"

            sp_dma(gate1, modr[:, 2 * D:3 * D], writes=["gate1"], key="c0")
            sp_dma(gm2, modr[:, 4 * D:5 * D], writes=["gm2"], key="c0")
            sp_dma(sh2, modr[:, 3 * D:4 * D], writes=["sh2"], key="c0")
            sp_dma(wr, w_r[l].rearrange("(k p) n -> p k n", p=128), writes=["wr"], key="c0")
            sp_dma(brr, b_r_rep[l], writes=["brr"], key="c0")
            memset("dve", carry, 0.0, ["carry"])
            memset("dve", zt, 0.0, ["zt"])
            for r in range(E * CAP // 128):
                sp_dma(x_buf[r * 128:(r + 1) * 128, :], zt, reads=["zt"], writes=["xbuf"], key="zx")
            gatev = w_in_b[l % 2].rearrange("(k p) n -> p k n", p=128)
            wbv = w_br_b[l % 2].rearrange("(n h p) d -> p n h d", p=128, h=4)
            wov = w_out_b[l % 2].rearrange("(k p) n -> p k n", p=128)
            yTv = yT.rearrange("(c p) s -> p c s", p=128)
            NJS = D // 256
            for g in range(NG):
                ts0 = g * 512
                sp_dma(RA, hTv[:, :, ts0:ts0 + 512], writes=["RA"], key="RA")
                sp_dma(RB, yTv[:, :, ts0:ts0 + 512], writes=["RB"], key="RB")
                for js in range(NJS):
                    for n in range(4):
                        bi = (js * 4 + n) % 3
                        pool_dma(gsl[bi], gatev[:, :, c.c_gate + n * D + js * 256:c.c_gate + n * D + (js + 1) * 256], writes=[f"gsl{bi}"], key=f"gsl{bi}")
                        pool_dma(bsl[bi], wbv[:, n, :, js * 256:(js + 1) * 256], writes=[f"bsl{bi}"], key=f"bsl{bi}")
                        for jj in range(2):
                            pg, pp = (0, 1) if jj == 0 else (2, 3)
                            for k in range(KD):
                                mm(PS[pg][:, :], gsl[bi][:, k, jj * 128:(jj + 1) * 128], RA[:, k, :], k == 0, k == KD - 1, [f"gsl{bi}", "RA"], [PSK[pg]])
                            for hh in range(4):
                                mm(PS[pp][:, :], bsl[bi][:, hh, jj * 128:(jj + 1) * 128], RB[:, n * 4 + hh, :], hh == 0, hh == 3, [f"bsl{bi}", "RB"], [PSK[pp]])
                            act(sg[jj], PS[pg][:, :], AF.Sigmoid, [PSK[pg]], [f"sg{jj}"])
                            j = js * 2 + jj
                            if n == 0:
                                tt("dve", acc[jj], sg[jj], PS[pp][:, :], ALU.mult, [f"sg{jj}", PSK[pp]], [f"acc{jj}"])
                            else:
                                tt("dve", mtmp, sg[jj], PS[pp][:, :], ALU.mult, [f"sg{jj}", PSK[pp]], ["mtmp"])
                                if n < 3:
                                    tt("dve", acc[jj], acc[jj], mtmp, ALU.add, [f"acc{jj}", "mtmp"], [f"acc{jj}"])
                                else:
                                    tt("dve", mrg[:, j, :], acc[jj], mtmp, ALU.add, [f"acc{jj}", "mtmp"], ["mrg"])
                for t4 in range(4):
                    tix = g * 4 + t4
                    sp_dma(xtl[t4], xsrc[tix * 128:(tix + 1) * 128, :], writes=[xkeys[t4], f"x{t4}"], key="xl")
                for js in range(NJS):
                    bi = js % 2
                    pool_dma(wos[bi], wov[:, :, js * 256:(js + 1) * 256], writes=[f"wos{bi}"], key=f"wos{bi}")
                    for t4 in range(4):
                        psi = 4 + (js * 4 + t4) % 3
                        for k in range(KD):
                            mm(PS[psi][:, 0:256], mrg[:, k, t4 * 128:(t4 + 1) * 128], wos[bi][:, k, :], k == 0, k == KD - 1, ["mrg", f"wos{bi}"], [PSK[psi]])
                        sl = slice(js * 256, (js + 1) * 256)
                        tt("dve", xtmp, PS[psi][:, 0:256], gate1[:, sl], ALU.mult, [PSK[psi], "gate1"], ["xtmp"])
                        tt("dve", xtl[t4][:, sl], xtl[t4][:, sl], xtmp, ALU.add, [f"x{t4}", "xtmp"], [f"x{t4}"])
                for t4 in range(4):
                    tix = g * 4 + t4
                    xk = [f"x{t4}", xkeys[t4]]
                    sp_dma(xmid[tix * 128:(tix + 1) * 128, :], xtl[t4], reads=xk, key="xs")
                    memset("dve", sm, 0.0, ["sm"])
                    act(h2f, xtl[t4], AF.Square, xk, ["h2f", "sm"], accum_out=sm[:, 0:1])
                    rstd_from_ssq(sm[:, 0:1], D, ["sm"])
                    stt("dve", h2f, xtl[t4], sm[:, 0:1], gm2, ALU.mult, ALU.mult, xk + ["sm", "gm2"], ["h2f"])
                    tt("dve", h2f, h2f, sh2, ALU.add, ["h2f", "sh2"], ["h2f"])
                    cp("act", h2b, h2f, ["h2f"], ["h2b"])
                    sp_dma(h2[tix * 128:(tix + 1) * 128, :], h2b, reads=["h2b"], key="h2st")
                    for k4 in range(0, KD, 4):
                        nk = min(4, KD - k4)
                        for kk in range(nk):
                            k = k4 + kk
                            P.op("pe", lambda e, o=PS[0][:, kk * 128:(kk + 1) * 128], i=h2f[:, k * 128:(k + 1) * 128]: e.transpose(out=o, in_=i, identity=identf),
                                 reads=["h2f", "cst"], writes=[PSK[0]])
                        cp("act" if (k4 // 4) % 2 == 0 else "dve", h2T[:, k4:k4 + nk, :], PS[0][:, 0:nk * 128].rearrange("p (a b) -> p a b", a=nk), [PSK[0]], ["h2T"])
                    for k in range(KD):
                        mm(PS[1][:, 0:36], h2T[:, k, :], wr[:, k, :], k == 0, k == KD - 1, ["h2T", "wr"], [PSK[1]])
                    tt("dve", lg, PS[1][:, 0:36], brr, ALU.add, [PSK[1], "brr"], ["lg"])
                    R = ["lg", "sm", "esel", "esel2", "oh", "goh", "gex", "A1", "posC", "ptmp", "slf"]
                    P.op("dve", lambda e: e.reduce_max(out=sm[:, 1:2], in_=lg[:, 0:4], axis=AX.X), reads=["lg"], writes=["sm"])
                    ts("dve", goh, lg[:, 0:4], sm[:, 1:2], 0.0, ALU.is_equal, ALU.add, ["lg", "sm"], ["goh"])
                    ts("dve", sm[:, 2:3], sm[:, 1:2], -1.0, 0.0, ALU.mult, ALU.add, ["sm"], ["sm"])
                    act(gex, lg[:, 0:4], AF.Exp, ["lg", "sm"], ["gex", "sm"], bias=sm[:, 2:3], accum_out=sm[:, 3:4])
                    P.op("dve", lambda e: e.reciprocal(out=sm[:, 4:5], in_=sm[:, 3:4]), reads=["sm"], writes=["sm"])
                    ts("dve", esel, lg[:, 4:12], goh[:, 0:1], 0.0, ALU.mult, ALU.add, ["lg", "goh"], ["esel"])
                    for gi in range(1, 4):
                        stt("dve", esel, lg[:, 4 + gi * 8:12 + gi * 8], goh[:, gi:gi + 1], esel, ALU.mult, ALU.add, ["lg", "goh", "esel"], ["esel"])
                    P.op("dve", lambda e: e.reduce_max(out=sm[:, 5:6], in_=esel, axis=AX.X), reads=["esel"], writes=["sm"])
                    ts("dve", oh[:, 0, :], esel, sm[:, 5:6], 0.0, ALU.is_equal, ALU.add, ["esel", "sm"], ["oh"])
                    stt("dve", esel2, oh[:, 0, :], -1e30, esel, ALU.mult, ALU.add, ["oh", "esel"], ["esel2"])
                    P.op("dve", lambda e: e.reduce_max(out=sm[:, 6:7], in_=esel2, axis=AX.X), reads=["esel2"], writes=["sm"])
                    ts("dve", oh[:, 1, :], esel2, sm[:, 6:7], 0.0, ALU.is_equal, ALU.add, ["esel2", "sm"], ["oh"])
                    tt("dve", sm[:, 7:8], sm[:, 6:7], sm[:, 5:6], ALU.subtract, ["sm"], ["sm"])
                    act(sm[:, 8:9], sm[:, 7:8], AF.Exp, ["sm"], ["sm"])
                    ts("dve", sm[:, 9:10], sm[:, 8:9], 1.0, 0.0, ALU.add, ALU.add, ["sm"], ["sm"])
                    P.op("dve", lambda e: e.reciprocal(out=sm[:, 9:10], in_=sm[:, 9:10]), reads=["sm"], writes=["sm"])
                    tt("dve", WTS[:, tix, 0:1], sm[:, 9:10], sm[:, 4:5], ALU.mult, ["sm"], ["WTS"])
                    tt("dve", WTS[:, tix, 1:2], WTS[:, tix, 0:1], sm[:, 8:9], ALU.mult, ["sm", "WTS"], ["WTS"])
                    for kk in range(2):
                        for gi in range(4):
                            ts("dve", A1[:, kk, gi * 8:(gi + 1) * 8], oh[:, kk, :], goh[:, gi:gi + 1], 0.0, ALU.mult, ALU.add, ["oh", "goh"], ["A1"])
                    tt("dve", Ab, A1[:, 0, :], A1[:, 1, :], ALU.add, ["A1"], ["Ab"])
                    mm(PS[2][:, 0:32], ltb, Ab, True, True, ["cstb", "Ab"], [PSK[2]])
                    mm(PS[3][:, 0:32], onesb, Ab, True, True, ["cstb", "Ab"], [PSK[3]])
                    tt("dve", posC, PS[2][:, 0:32], carry, ALU.add, [PSK[2], "carry"], ["posC"])
                    tt("dve", carry, carry, PS[3][:, 0:32], ALU.add, ["carry", PSK[3]], ["carry"])
                    ts("dve", ptmp, posC, float(CAP), 1.0e7, ALU.is_ge, ALU.mult, ["posC"], ["ptmp"])
                    tt("dve", posC, posC, ptmp, ALU.add, ["posC", "ptmp"], ["posC"])
                    tt("dve", posC, posC, ecap, ALU.add, ["posC", "ecap"], ["posC"])
                    for kk in range(2):
                        tt("dve", ptmp, A1[:, kk, :], posC, ALU.mult, ["A1", "posC"], ["ptmp"])
                        P.op("dve", lambda e, kk=kk: e.reduce_sum(out=slf[:, kk:kk + 1], in_=ptmp, axis=AX.X), reads=["ptmp"], writes=["slf"])
                    cp("dve", SLOT[:, tix, :], slf, ["slf"], ["SLOT"])
                    for kk in range(2):
                        P.op("pool", lambda e, kk=kk, tix=tix: e.indirect_dma_start(out=x_buf[:, :], out_offset=bass.IndirectOffsetOnAxis(ap=SLOT[:, tix, kk:kk + 1], axis=0),
                                                                              in_=h2b, in_offset=None, bounds_check=regs["bk"], oob_is_err=False),
                             reads=["h2b", "SLOT"], writes=["xbuf"], dma_key="scat")
            sp_dma(cnt_out[l], carry, reads=["carry"], key="cnt")
            P.barrier()

            A.reset()
            xrow = [A.alloc([D], BF16) for _ in range(2)]
            XT = A.alloc([KD, CAP], BF16)
            FW = min(512, DE)
            wgs = [A.alloc([KD, FW], BF16) for _ in range(2)]
            wus = [A.alloc([KD, FW], BF16) for _ in range(2)]
            sgl = [A.alloc([min(CAP, 512)], F32) for _ in range(2)]
            HT = A.alloc([KF, CAP], BF16)
            WD = min(512, D)
            wds = [A.alloc([KF, WD], BF16) for _ in range(3)]
            ysb = [A.alloc([WD], F32) for _ in range(3)]
            NCT = CAP // 128
            yrot = 0
            for e_ in range(E):
                for ct in range(NCT):
                    b = ct % 2
                    sp_dma(xrow[b], x_buf[e_ * CAP + ct * 128:e_ * CAP + (ct + 1) * 128, :], writes=[f"xrow{b}"], key=f"xrow{b}")
                    for k4 in range(0, KD, 8):
                        nk = min(8, KD - k4)
                        for kk in range(nk):
                            k = k4 + kk
                            P.op("pe", lambda e, o=PSB[:, kk * 128:(kk + 1) * 128], i=xrow[b][:, k * 128:(k + 1) * 128]: e.transpose(out=o, in_=i, identity=identb),
                                 reads=[f"xrow{b}", "cstb"], writes=["psb"])
                        cp("act" if (k4 // 8) % 2 == 0 else "dve", XT[:, k4:k4 + nk, ct * 128:(ct + 1) * 128],
                           PSB[:, 0:nk * 128].rearrange("p (a b) -> p a b", a=nk), ["psb"], ["XT"])
                wgv = w_gate[l, e_].rearrange("(k p) n -> p k n", p=128)
                wuv = w_up[l, e_].rearrange("(k p) n -> p k n", p=128)
                wdv = w_down[l, e_].rearrange("(k p) n -> p k n", p=128)
                ranges = [(r0, min(r0 + 512, CAP)) for r0 in range(0, CAP, 512)]
                for fs in range(DE // FW):
                    bi = fs % 2
                    pool_dma(wgs[bi], wgv[:, :, fs * FW:(fs + 1) * FW], writes=[f"wgs{bi}"], key=f"wgs{bi}")
                    pool_dma(wus[bi], wuv[:, :, fs * FW:(fs + 1) * FW], writes=[f"wus{bi}"], key=f"wus{bi}")
                    for fc in range(FW // 128):
                        f = fs * (FW // 128) + fc
                        for ri, (r0, r1) in enumerate(ranges):
                            par = (fc * len(ranges) + ri) % 2
                            pg, pu_ = (0, 1) if par == 0 else (2, 3)
                            w_ = r1 - r0
                            for k in range(KD):
                                mm(PS[pg][:, 0:w_], wgs[bi][:, k, fc * 128:(fc + 1) * 128], XT[:, k, r0:r1], k == 0, k == KD - 1, [f"wgs{bi}", "XT"], [PSK[pg]])
                            for k in range(KD):
                                mm(PS[pu_][:, 0:w_], wus[bi][:, k, fc * 128:(fc + 1) * 128], XT[:, k, r0:r1], k == 0, k == KD - 1, [f"wus{bi}", "XT"], [PSK[pu_]])
                            act(sgl[par][:, 0:w_], PS[pg][:, 0:w_], AF.Silu, [PSK[pg]], [f"sgl{par}"])
                            tt("dve", HT[:, f, r0:r1], sgl[par][:, 0:w_], PS[pu_][:, 0:w_], ALU.mult, [f"sgl{par}", PSK[pu_]], ["HT"])
                for dsb in range(D // WD):
                    bi = (e_ * (D // WD) + dsb) % 3
                    pool_dma(wds[bi], wdv[:, :, dsb * WD:(dsb + 1) * WD], writes=[f"wds{bi}"], key=f"wds{bi}")
                    for ct in range(NCT):
                        psi = 4 + (dsb * NCT + ct) % 3
                        for f in range(KF):
                            mm(PS[psi][:, 0:WD], HT[:, f, ct * 128:(ct + 1) * 128], wds[bi][:, f, :], f == 0, f == KF - 1, ["HT", f"wds{bi}"], [PSK[psi]])
                        yi = yrot % 3
                        yrot += 1
                        cp("act" if yrot % 2 == 0 else "dve", ysb[yi], PS[psi][:, 0:WD], [PSK[psi]], [f"ysb{yi}"])
                        sp_dma(y_buf[e_ * CAP + ct * 128:e_ * CAP + (ct + 1) * 128, dsb * WD:(dsb + 1) * WD], ysb[yi], reads=[f"ysb{yi}"], key=f"ysb{yi}")
            P.barrier()

            A.reset()
            gate2 = A.alloc([D], F32)
            gfin = A.alloc([D], F32)
            r1 = [A.alloc([D], F32) for _ in range(2)]
            r2 = [A.alloc([D], F32) for _ in range(2)]
            xm = [A.alloc([D], F32) for _ in range(2)]
            mo = A.alloc([D], F32)
            sq2 = A.alloc([D], BF16)
            fs_ = A.alloc([4], F32)
            wz = A.alloc([2], F32)
            sp_dma(gate2, modr[:, 5 * D:6 * D], writes=["gate2"], key="f0")
            if last:
                sp_dma(gfin, gf_rep[:, :], writes=["gfin"], key="f0")
            for b in range(2):
                memset("dve", r1[b], 0.0, [f"r1{b}"])
                memset("dve", r2[b], 0.0, [f"r2{b}"])
            for tix in range(NT):
                b = tix % 2
                sp_dma(xm[b], xmid[tix * 128:(tix + 1) * 128, :], writes=[f"xm{b}"], key=f"xm{b}")
                for (rr, rk, kk) in ((r1[b], f"r1{b}", 0), (r2[b], f"r2{b}", 1)):
                    P.op("pool", lambda e, rr=rr, kk=kk, tix=tix: e.indirect_dma_start(out=rr, out_offset=None, in_=y_buf[:, :],
                                                                                   in_offset=bass.IndirectOffsetOnAxis(ap=SLOT[:, tix, kk:kk + 1], axis=0),
                                                                                   bounds_check=regs["bk"], oob_is_err=False),
                         reads=["SLOT"], writes=[rk], dma_key=rk)
                P.op("dve", lambda e, tix=tix: e.tensor_copy(out=wz, in_=SLOT[:, tix, :]), reads=["SLOT"], writes=["wz"])
                ts("dve", wz, wz, float(E * CAP), 0.0, ALU.is_lt, ALU.add, ["wz"], ["wz"])
                tt("dve", wz, wz, WTS[:, tix, :], ALU.mult, ["wz", "WTS"], ["wz"])
                ts("dve", mo, r1[b], wz[:, 0:1], 0.0, ALU.mult, ALU.add, [f"r1{b}", "wz"], ["mo"])
                stt("dve", mo, r2[b], wz[:, 1:2], mo, ALU.mult, ALU.add, [f"r2{b}", "wz", "mo"], ["mo"])
                tt("dve", mo, mo, gate2, ALU.mult, ["mo", "gate2"], ["mo"])
                tt("dve", xm[b], xm[b], mo, ALU.add, [f"xm{b}", "mo"], [f"xm{b}"])
                if not last:
                    sp_dma(xres[tix * 128:(tix + 1) * 128, :], xm[b], reads=[f"xm{b}"], key=f"xo{b}")
                else:
                    memset("dve", fs_, 0.0, ["fs"])
                    act(sq2, xm[b], AF.Square, [f"xm{b}"], ["sq2", "fs"], accum_out=fs_[:, 0:1])
                    rstd_from_ssq(fs_[:, 0:1], D, ["fs"])
                    stt("dve", xm[b], xm[b], fs_[:, 0:1], gfin, ALU.mult, ALU.mult, [f"xm{b}", "fs", "gfin"], [f"xm{b}"])
                    sp_dma(out[tix * 128:(tix + 1) * 128, :], xm[b], reads=[f"xm{b}"], key=f"xo{b}")
            P.barrier()
        P.emit()
    return nc


def host_consts(cfg):
    p = np.arange(128)[:, None]
    f = np.arange(128)[None, :]
    cst = np.zeros((128, 7, 128), np.float32)
    cst[:, 0] = (p == f)
    cst[:, 1] = (p <= f)
    cst[:, 2] = (p < f)
    cst[:, 3] = (p >= f)
    cst[:, 4] = 1.0
    cst[:, 5] = (p == 127)
    half = cfg.RD // 2
    inv = np.power(np.float32(10000.0), (-2.0 * np.arange(half, dtype=np.float32) / cfg.RD).astype(np.float32)).astype(np.float32)
    ang = np.arange(cfg.S, dtype=np.float32)[:, None] * inv[None, :]
    cos = np.cos(ang).astype(np.float32).T
    sin = np.sin(ang).astype(np.float32).T
    rope = np.stack([np.concatenate([cos, cos], 0), np.concatenate([-sin, sin], 0)], 0).astype(np.float32)
    t = np.arange(cfg.S)
    ic = np.stack([1.0 / np.minimum(t + 1, w) for w in (2, 4, 8, 16)], 0).astype(np.float32)
    invcnt = np.ascontiguousarray(np.broadcast_to(ic[None], (128, 4, cfg.S)))
    ecap = np.ascontiguousarray(np.broadcast_to((np.arange(32, dtype=np.float32) * cfg.CAP)[None], (128, 32)))
    return cst, rope, invcnt, ecap


def _rep(v):
    return np.ascontiguousarray(np.broadcast_to(np.asarray(v, np.float32)[..., None, :], v.shape[:-1] + (128, v.shape[-1])))


def host_prepare(cfg, inp):
    c = cfg
    D, L, QL, KVL = c.D, c.L, c.QL, c.KVL
    o_fq, o_fk, o_fv, o_ff = 0, 512, 1024, 1536
    o_sq, o_sk, o_sv = 1540, 2052, 2564
    o_cq = 3076
    o_ckv = o_cq + QL
    o_kr = o_ckv + KVL
    o_pu = o_kr + 64
    o_gate = o_pu + 512
    ar = np.arange
    cols = np.concatenate([
        o_fq + ar(512), o_fk + ar(512), o_sq + ar(512), o_sk + ar(512),
        o_cq + ar(QL), o_ckv + ar(KVL), o_pu + ar(512),
        o_kr + ar(64), o_kr + np.concatenate([ar(32, 64), ar(0, 32)]),
        o_fv + ar(512), o_sv + ar(512), o_ff + ar(4), o_gate + ar(4 * D)])
    assert cols.shape[0] == c.NP
    w_in_p = np.ascontiguousarray(np.take(np.asarray(inp["w_in"], np.float32), cols, axis=2))
    uq_cols = [h * 192 + ar(128) for h in range(4)]
    for h in range(4):
        uq_cols.append(h * 192 + 128 + ar(64))
        uq_cols.append(h * 192 + 128 + np.concatenate([ar(32, 64), ar(0, 32)]))
    w_uq_p = np.ascontiguousarray(np.take(np.asarray(inp["w_uq"], np.float32), np.concatenate(uq_cols), axis=2))
    ukv_cols = np.concatenate([h * 256 + ar(128) for h in range(4)] + [h * 256 + 128 + ar(128) for h in range(4)])
    w_ukv_p = np.ascontiguousarray(np.take(np.asarray(inp["w_ukv"], np.float32), ukv_cols, axis=2))
    cst, rope, invcnt, ecap = host_consts(c)
    f32 = lambda a: np.ascontiguousarray(np.asarray(a, np.float32))
    shared = {
        "w_mod": f32(inp["w_mod"]), "b_mod_rep": _rep(f32(inp["b_mod"])),
        "g1_rep": _rep(f32(inp["g_norm1"])), "g2_rep": _rep(f32(inp["g_norm2"])),
        "gf_rep": _rep(f32(inp["g_final"])),
        "w_in_p": w_in_p, "bf_rep": _rep(f32(inp["b_forget"])),
        "gq": np.ascontiguousarray(f32(inp["g_q_norm"]).reshape(L, c.KQ, 128).transpose(0, 2, 1)),
        "w_uq_p": w_uq_p,
        "gkv": np.ascontiguousarray(f32(inp["g_kv_norm"]).reshape(L, c.KK, 128).transpose(0, 2, 1)),
        "w_ukv_p": w_ukv_p, "w_pool": f32(inp["w_pool"]),
        "pscale": np.ascontiguousarray(f32(inp["pool_scale"]).reshape(L, 4, 128).transpose(0, 2, 1)),
        "w_branch": f32(inp["w_branch"]), "w_out": f32(inp["w_out"]),
        "w_r": np.ascontiguousarray(np.concatenate([f32(inp["w_route_group"]), f32(inp["w_route_expert"])], axis=2)),
        "b_r_rep": _rep(np.concatenate([f32(inp["b_route_group"]), f32(inp["b_route_expert"])], axis=1)),
        "w_gate": f32(inp["w_gate"]), "w_up": f32(inp["w_up"]), "w_down": f32(inp["w_down"]),
        "consts": cst, "rope": rope, "invcnt": invcnt, "ecap": ecap,
    }
    x = f32(inp["x"])
    cc = f32(inp["c"])
    maps = []
    for b in range(x.shape[0]):
        m = dict(shared)
        m["x"] = np.ascontiguousarray(x[b])
        m["cT"] = np.ascontiguousarray(cc[b].reshape(c.KD, 128).T)
        maps.append(m)
    return maps


def kernel(**inputs):
    cfg = Cfg()
    maps = host_prepare(cfg, inputs)
    nc = build_program(cfg)
    res = run_bass_kernel_spmd(nc, maps, core_ids=list(range(len(maps))))
    try:
        mx = max(float(np.asarray(r["cnt"]).max()) for r in res.results)
        print(f"[kernel] max tokens routed to one expert within one sequence: {mx:.0f} (capacity {cfg.CAP})", flush=True)
    except Exception:
        pass
    return np.stack([np.asarray(r["out"], np.float32) for r in res.results], 0)
```
